# Optimizing a Trainium2 kernel written in Bass

```python
import jax, jax.numpy as jnp
from jax import lax
import numpy as np

D_MODEL = 1024
BATCH = 16
SEQ = 2048
DEPTH = 2

N_MIXERS = 2
EPS = 1e-6
D_FF = 2816
POOL_WINDOWS = (2, 4, 8, 16)
N_POOL_GROUPS = 4
POOL_GROUP = D_MODEL // N_POOL_GROUPS
N_HEADS = 16
HEAD_DIM = D_MODEL // N_HEADS
N_KV_GROUPS = 4
HEADS_PER_GROUP = N_HEADS // N_KV_GROUPS
ROPE_DIM = HEAD_DIM // 4
ROPE_THETA = 500000.0
CMP_BLOCK = 32
CMP_STRIDE = 16
CMP_HIDDEN = 256
SEL_BLOCK = 64
N_SELECT = 8
SEL_Q_BLOCK = 32
WINDOW = 256
WIN_Q_BLOCK = 128
Q_WIDTH = N_HEADS * HEAD_DIM
KV_WIDTH = N_KV_GROUPS * HEAD_DIM
GATE_WIDTH = 3 * N_HEADS
IN_WIDTH = Q_WIDTH + 6 * KV_WIDTH + GATE_WIDTH
SPLIT_POINTS = tuple(Q_WIDTH + i * KV_WIDTH for i in range(7))
N_POOL_LAYERS = (DEPTH + N_MIXERS - 1) // N_MIXERS
N_NSA_LAYERS = DEPTH // N_MIXERS
NEG_INF = -1e30

kernel_name = "hybrid_pool_nsa_macaron"


def rms_norm(x, g):
    xf = x.astype(jnp.float32)
    y = xf * lax.rsqrt(jnp.mean(xf * xf, axis=-1, keepdims=True) + EPS)
    return (y * g.astype(jnp.float32)).astype(x.dtype)


def swiglu(x, w_gate, w_up, w_down):
    return (jax.nn.silu(x @ w_gate) * (x @ w_up)) @ w_down


def rope_tables(seq):
    inv = 1.0 / (ROPE_THETA ** (jnp.arange(0, ROPE_DIM, 2, dtype=jnp.float32) / ROPE_DIM))
    ang = jnp.arange(seq, dtype=jnp.float32)[:, None] * inv[None, :]
    return jnp.cos(ang), jnp.sin(ang)


def partial_rope(t, cos, sin):
    half = ROPE_DIM // 2
    c = cos[None, :, None, :].astype(t.dtype)
    s = sin[None, :, None, :].astype(t.dtype)
    x1, x2, rest = t[..., :half], t[..., half:ROPE_DIM], t[..., ROPE_DIM:]
    return jnp.concatenate([x1 * c - x2 * s, x1 * s + x2 * c, rest], axis=-1)


def pool_mixer(h, w_grp, b_grp, scale):
    B, S, D = h.shape
    hf = h.astype(jnp.float32)
    cum = jnp.concatenate([jnp.zeros((B, 1, D), jnp.float32), jnp.cumsum(hf, axis=1)], axis=1)
    end = jnp.arange(1, S + 1)
    outs = []
    for g, w in enumerate(POOL_WINDOWS):
        sl = slice(g * POOL_GROUP, (g + 1) * POOL_GROUP)
        lo = jnp.maximum(end - w, 0)
        cnt = (end - lo).astype(jnp.float32)[None, :, None]
        mean = (cum[:, 1:, sl] - cum[:, lo, sl]) / cnt
        outs.append(mean - hf[:, :, sl])
    d = jnp.stack(outs, axis=2).astype(h.dtype)
    y = jnp.einsum('bsgc,gce->bsge', d, w_grp) + b_grp
    return y.reshape(B, S, D) * scale


def compress_blocks(blk, pos_emb, w1, w2):
    B, Nc = blk.shape[:2]
    z = (blk + pos_emb[None, None, :, None, :]).transpose(0, 1, 3, 2, 4)
    z = z.reshape(B, Nc, N_KV_GROUPS, CMP_BLOCK * HEAD_DIM)
    return jax.nn.silu(z @ w1) @ w2


def nsa_mixer(h, w_in, pos_k, pos_v, wk1, wk2, wv1, wv2, w_o, cos, sin):
    B, S, _ = h.shape
    G, R, dh = N_KV_GROUPS, HEADS_PER_GROUP, HEAD_DIM
    scale = HEAD_DIM ** -0.5
    proj = h @ w_in
    q, k_c, v_c, k_s, v_s, k_w, v_w, gate = jnp.split(proj, SPLIT_POINTS, axis=-1)
    q = q.reshape(B, S, N_HEADS, dh)
    q_nope = q.reshape(B, S, G, R, dh)
    q_rot = partial_rope(q, cos, sin).reshape(B, S, G, R, dh)
    k_c, v_c, k_s, v_s, k_w, v_w = [t.reshape(B, S, G, dh) for t in (k_c, v_c, k_s, v_s, k_w, v_w)]
    k_s = partial_rope(k_s, cos, sin)
    k_w = partial_rope(k_w, cos, sin)
    pos = jnp.arange(S)

    n_cmp = (S - CMP_BLOCK) // CMP_STRIDE + 1
    blk_tok = jnp.arange(n_cmp)[:, None] * CMP_STRIDE + jnp.arange(CMP_BLOCK)[None, :]
    k_cmp = compress_blocks(k_c[:, blk_tok], pos_k, wk1, wk2)
    v_cmp = compress_blocks(v_c[:, blk_tok], pos_v, wv1, wv2)
    blk_end = jnp.arange(n_cmp) * CMP_STRIDE + CMP_BLOCK - 1
    cmp_mask = blk_end[None, :] <= pos[:, None]
    s_c = jnp.einsum('bsgrd,bngd->bgrsn', q_nope, k_cmp).astype(jnp.float32) * scale
    p_c = jax.nn.softmax(jnp.where(cmp_mask, s_c, NEG_INF), axis=-1)
    p_c = p_c * jnp.any(cmp_mask, axis=-1)[:, None].astype(jnp.float32)
    o_cmp = jnp.einsum('bgrsn,bngd->bsgrd', p_c.astype(v_cmp.dtype), v_cmp)

    n_sel_blk = S // SEL_BLOCK
    n_pick = min(N_SELECT, n_sel_blk)
    ci = jnp.arange(n_cmp) * CMP_STRIDE
    sj = jnp.arange(n_sel_blk) * SEL_BLOCK
    overlap = ((ci[:, None] < sj[None, :] + SEL_BLOCK) &
               (ci[:, None] + CMP_BLOCK > sj[None, :])).astype(jnp.float32)
    imp = jnp.einsum('bgrsn,nj->bgsj', p_c, overlap)
    q_blk = pos // SEL_BLOCK
    j = jnp.arange(n_sel_blk)[None, :]
    future = j > q_blk[:, None]
    forced = (j == 0) | (j == q_blk[:, None]) | (j == q_blk[:, None] - 1)
    imp = jnp.where(forced, jnp.inf, jnp.where(future, -jnp.inf, imp))
    _, sel_idx = lax.top_k(imp, n_pick)

    k_blocks = k_s.reshape(B, n_sel_blk, SEL_BLOCK, G, dh).transpose(0, 3, 1, 2, 4)
    v_blocks = v_s.reshape(B, n_sel_blk, SEL_BLOCK, G, dh).transpose(0, 3, 1, 2, 4)
    n_qs = S // SEL_Q_BLOCK
    b_ix = jnp.arange(B)[:, None, None]
    g_ix = jnp.arange(G)[None, :, None]

    def sel_block(args):
        qb, idx, start = args
        flat = idx.reshape(B, G, SEL_Q_BLOCK * n_pick)
        kb = k_blocks[b_ix, g_ix, flat].reshape(B, G, SEL_Q_BLOCK, n_pick * SEL_BLOCK, dh)
        vb = v_blocks[b_ix, g_ix, flat].reshape(B, G, SEL_Q_BLOCK, n_pick * SEL_BLOCK, dh)
        tok = (idx[..., None] * SEL_BLOCK + jnp.arange(SEL_BLOCK)).reshape(B, G, SEL_Q_BLOCK, n_pick * SEL_BLOCK)
        qpos = start + jnp.arange(SEL_Q_BLOCK)
        mask = (tok <= qpos[None, None, :, None])[:, :, None]
        s = jnp.einsum('bqgrd,bgqtd->bgrqt', qb, kb).astype(jnp.float32) * scale
        p = jax.nn.softmax(jnp.where(mask, s, NEG_INF), axis=-1).astype(vb.dtype)
        return jnp.einsum('bgrqt,bgqtd->bqgrd', p, vb)

    q_sel = jnp.moveaxis(q_rot.reshape(B, n_qs, SEL_Q_BLOCK, G, R, dh), 1, 0)
    idx_sel = jnp.moveaxis(sel_idx.reshape(B, G, n_qs, SEL_Q_BLOCK, n_pick), 2, 0)
    o_sel = lax.map(sel_block, (q_sel, idx_sel, jnp.arange(n_qs) * SEL_Q_BLOCK))
    o_sel = jnp.moveaxis(o_sel, 0, 1).reshape(B, S, G, R, dh)

    n_qw = S // WIN_Q_BLOCK
    k_pad = jnp.pad(k_w, ((0, 0), (WINDOW, 0), (0, 0), (0, 0)))
    v_pad = jnp.pad(v_w, ((0, 0), (WINDOW, 0), (0, 0), (0, 0)))
    band = WIN_Q_BLOCK + WINDOW

    def win_block(args):
        qb, start = args
        kb = lax.dynamic_slice_in_dim(k_pad, start, band, axis=1)
        vb = lax.dynamic_slice_in_dim(v_pad, start, band, axis=1)
        qpos = start + jnp.arange(WIN_Q_BLOCK)
        kpos = start - WINDOW + jnp.arange(band)
        diff = qpos[:, None] - kpos[None, :]
        mask = (diff >= 0) & (diff < WINDOW) & (kpos[None, :] >= 0)
        s = jnp.einsum('bqgrd,btgd->bgrqt', qb, kb).astype(jnp.float32) * scale
        p = jax.nn.softmax(jnp.where(mask, s, NEG_INF), axis=-1).astype(vb.dtype)
        return jnp.einsum('bgrqt,btgd->bqgrd', p, vb)

    q_win = jnp.moveaxis(q_rot.reshape(B, n_qw, WIN_Q_BLOCK, G, R, dh), 1, 0)
    o_win = lax.map(win_block, (q_win, jnp.arange(n_qw) * WIN_Q_BLOCK))
    o_win = jnp.moveaxis(o_win, 0, 1).reshape(B, S, G, R, dh)

    gts = jax.nn.sigmoid(gate.reshape(B, S, 3, G, R, 1))
    o = gts[:, :, 0] * o_cmp + gts[:, :, 1] * o_sel + gts[:, :, 2] * o_win
    return o.reshape(B, S, Q_WIDTH) @ w_o


def setup_inputs(seed: int = 0) -> dict:
    key = jax.random.key(seed)
    ks = jax.random.split(key, 20)
    nrm = lambda k, shape, fan_in: jax.random.normal(k, shape, jnp.float32) * (fan_in ** -0.5)
    return {
        "x": jax.random.normal(ks[0], (BATCH, SEQ, D_MODEL), jnp.float32),
        "norm_gains": 1.0 + 0.05 * jax.random.normal(ks[1], (DEPTH, 6, D_MODEL), jnp.float32),
        "ffn_w_gate": nrm(ks[2], (DEPTH, 2, D_MODEL, D_FF), D_MODEL),
        "ffn_w_up": nrm(ks[3], (DEPTH, 2, D_MODEL, D_FF), D_MODEL),
        "ffn_w_down": nrm(ks[4], (DEPTH, 2, D_FF, D_MODEL), D_FF),
        "pool_w": nrm(ks[5], (N_POOL_LAYERS, N_POOL_GROUPS, POOL_GROUP, POOL_GROUP), POOL_GROUP),
        "pool_b": 0.01 * jax.random.normal(ks[6], (N_POOL_LAYERS, N_POOL_GROUPS, POOL_GROUP), jnp.float32),
        "pool_scale": 0.5 + 0.1 * jax.random.normal(ks[7], (N_POOL_LAYERS, D_MODEL), jnp.float32),
        "nsa_w_in": nrm(ks[8], (N_NSA_LAYERS, D_MODEL, IN_WIDTH), D_MODEL),
        "nsa_cmp_pos_k": 0.1 * jax.random.normal(ks[9], (N_NSA_LAYERS, CMP_BLOCK, HEAD_DIM), jnp.float32),
        "nsa_cmp_pos_v": 0.1 * jax.random.normal(ks[10], (N_NSA_LAYERS, CMP_BLOCK, HEAD_DIM), jnp.float32),
        "nsa_cmp_wk1": nrm(ks[11], (N_NSA_LAYERS, CMP_BLOCK * HEAD_DIM, CMP_HIDDEN), CMP_BLOCK * HEAD_DIM),
        "nsa_cmp_wk2": nrm(ks[12], (N_NSA_LAYERS, CMP_HIDDEN, HEAD_DIM), CMP_HIDDEN),
        "nsa_cmp_wv1": nrm(ks[13], (N_NSA_LAYERS, CMP_BLOCK * HEAD_DIM, CMP_HIDDEN), CMP_BLOCK * HEAD_DIM),
        "nsa_cmp_wv2": nrm(ks[14], (N_NSA_LAYERS, CMP_HIDDEN, HEAD_DIM), CMP_HIDDEN),
        "nsa_w_o": nrm(ks[15], (N_NSA_LAYERS, Q_WIDTH, D_MODEL), Q_WIDTH),
    }


def reference(x, norm_gains, ffn_w_gate, ffn_w_up, ffn_w_down, pool_w, pool_b, pool_scale,
              nsa_w_in, nsa_cmp_pos_k, nsa_cmp_pos_v, nsa_cmp_wk1, nsa_cmp_wk2,
              nsa_cmp_wv1, nsa_cmp_wv2, nsa_w_o):
    cos, sin = rope_tables(x.shape[1])
    for i in range(DEPTH):
        g = norm_gains[i]
        f = swiglu(rms_norm(x, g[0]), ffn_w_gate[i, 0], ffn_w_up[i, 0], ffn_w_down[i, 0])
        x = x + 0.5 * rms_norm(f, g[1])
        h = rms_norm(x, g[2])
        j = i // N_MIXERS
        if i % N_MIXERS == 0:
            m = pool_mixer(h, pool_w[j], pool_b[j], pool_scale[j])
        else:
            m = nsa_mixer(h, nsa_w_in[j], nsa_cmp_pos_k[j], nsa_cmp_pos_v[j], nsa_cmp_wk1[j],
                          nsa_cmp_wk2[j], nsa_cmp_wv1[j], nsa_cmp_wv2[j], nsa_w_o[j], cos, sin)
        x = x + rms_norm(m, g[3])
        f = swiglu(rms_norm(x, g[4]), ffn_w_gate[i, 1], ffn_w_up[i, 1], ffn_w_down[i, 1])
        x = x + 0.5 * rms_norm(f, g[5])
    return x
```

```python
import contextlib
import numpy as np
import concourse.bass as bass
import concourse.mybir as mybir
from concourse.bass_utils import run_bass_kernel_spmd

F32 = mybir.dt.float32
BF16 = mybir.dt.bfloat16
AF = mybir.ActivationFunctionType
ALU = mybir.AluOpType
AX = mybir.AxisListType

ENGS = ["pe", "act", "dve", "pool", "sp"]
S = 2048
D = 1024
DFF = 2816
NT = S // 128
KC = D // 128
FC = DFF // 128
EPS = 1e-6
N_CORES = 8
SEQ_PER_CORE = 2


class Buf:
    __slots__ = ("name", "w", "r")

    def __init__(self, name):
        self.name = name
        self.w = None
        self.r = {}


class Prog:
    def __init__(self, nc):
        self.nc = nc
        self.q = {e: [] for e in ENGS}
        self.cnt = {}
        self.known = {e: {} for e in ENGS}

    def _waits(self, eng, toks):
        waits = []
        kn = self.known[eng]
        for key, val in toks:
            if kn.get(key, 0) >= val:
                continue
            kn[key] = val
            waits.append((key, val))
        return waits

    def _deps(self, eng, reads, writes, extra):
        deps = []
        for b in reads:
            if b.w is not None:
                deps.append(b.w)
        for b in writes:
            if b.w is not None:
                deps.append(b.w)
            deps.extend(b.r.values())
        deps.extend(t for t in extra if t is not None)
        return self._waits(eng, deps)

    def _mark(self, tok, reads, writes):
        key = tok[0]
        for b in reads:
            b.r[key] = tok
        for b in writes:
            b.w = tok
            b.r = {}

    def op(self, eng, fn, reads=(), writes=(), extra=()):
        waits = self._deps(eng, reads, writes, extra)
        key = "E_" + eng
        self.cnt[key] = self.cnt.get(key, 0) + 1
        tok = (key, self.cnt[key])
        self.q[eng].append((waits, fn, key, 1))
        self._mark(tok, reads, writes)
        return tok

    def dma(self, eng, stream, fn, reads=(), writes=(), extra=()):
        waits = self._deps(eng, reads, writes, extra)
        key = "D_" + stream
        self.cnt[key] = self.cnt.get(key, 0) + 16
        tok = (key, self.cnt[key])
        self.q[eng].append((waits, fn, key, 16))
        self._mark(tok, reads, writes)
        return tok

    def wait(self, eng, toks):
        self.q[eng].append((self._waits(eng, toks), None, None, 0))

    def barrier(self):
        toks = [(k, v) for k, v in self.cnt.items()]
        for e in ENGS:
            self.wait(e, toks)

    def emit(self):
        nc = self.nc
        with contextlib.ExitStack() as es:
            sems = {key: es.enter_context(nc.semaphore(key)) for key in self.cnt}
            block = es.enter_context(nc.Block())

            def run(e, name):
                for waits, fn, key, inc in self.q[name]:
                    for k, v in waits:
                        e.wait_ge(sems[k], v)
                    if fn is None:
                        continue
                    fn(e).then_inc(sems[key], inc)

            @block.tensor
            def _(e):
                run(e, "pe")

            @block.scalar
            def _(e):
                run(e, "act")

            @block.vector
            def _(e):
                run(e, "dve")

            @block.gpsimd
            def _(e):
                run(e, "pool")

            @block.sync
            def _(e):
                run(e, "sp")


def host_consts(Sx=S):
    c = {}
    c["ident"] = np.eye(128, dtype=np.float32)
    inv = np.zeros((128, 4, 16), np.float32)
    for g, w in enumerate((2, 4, 8, 16)):
        for t in range(16):
            inv[:, g, t] = 1.0 / min(t + 1, w)
    c["invcnt"] = inv.reshape(128, 64)
    invf = 1.0 / (500000.0 ** (np.arange(0, 16, 2, dtype=np.float32) / 16))
    ang = np.arange(Sx, dtype=np.float32)[:, None] * invf[None, :]
    c["cs"] = np.concatenate([np.cos(ang), np.sin(ang)], 1).astype(np.float32)
    n = np.arange(128)[:, None]
    sp = np.arange(Sx)[None, :]
    c["cmpmask"] = ((16 * n + 31 <= sp) & (n < 127)).astype(np.float32)
    j = np.arange(32)[None, :]
    qb = (np.arange(Sx) // 64)[:, None]
    forced = (j == 0) | (j == qb) | (j == qb - 1)
    future = j > qb
    c["addtbl"] = np.where(forced, 1e9, np.where(future, -1e9, 0.0)).astype(np.float32)
    a = np.arange(128)
    cm = np.zeros((128, 3, 128), np.float32)
    cm[:, 0, :] = (a[None, :] <= a[:, None])
    cm[:, 1, :] = (a[:, None] <= a[None, :])
    cm[:, 2, :] = (a[:, None] > a[None, :])
    c["cm3"] = cm.reshape(128, 384)
    ov = np.zeros((128, 33), np.float32)
    ov[:127, 0] = 1.0
    ci = np.arange(127)[:, None] * 16
    sj = np.arange(32)[None, :] * 64
    ov[:127, 1:] = ((ci < sj + 64) & (ci + 32 > sj)).astype(np.float32)
    c["ovl"] = ov
    return c


def const_shapes(Sx):
    return {"ident": [128, 128], "invcnt": [128, 64], "cs": [Sx, 16], "cmpmask": [128, Sx], "addtbl": [Sx, 32], "cm3": [128, 384], "ovl": [128, 33]}


def build_nc(n_seq=SEQ_PER_CORE, plan=None, S=S, nw=4):
    NT = S // 128
    if plan is None:
        plan = ["ffn00", "pool", "ffn01", "ffn10", "nsa", "ffn11"]
    nc = bass.Bass("TRN2", target_bir_lowering=False)

    def din(name, shape):
        return nc.dram_tensor(name, list(shape), F32, kind="ExternalInput").ap()

    x_d = din("x", [n_seq, S, D])
    gains_d = din("norm_gains", [12, D])
    wg_d = din("ffn_w_gate", [nw, D, DFF])
    wu_d = din("ffn_w_up", [nw, D, DFF])
    wd_d = din("ffn_w_down", [nw, DFF, D])
    poolw_d = din("pool_w", [4, 256, 256])
    poolb_d = din("pool_b", [D])
    pools_d = din("pool_scale", [D])
    win_d = din("nsa_w_in", [D, 2608])
    posk_d = din("nsa_cmp_pos_k", [32, 64])
    posv_d = din("nsa_cmp_pos_v", [32, 64])
    wk1_d = din("nsa_cmp_wk1", [2048, 256])
    wk2_d = din("nsa_cmp_wk2", [256, 64])
    wv1_d = din("nsa_cmp_wv1", [2048, 256])
    wv2_d = din("nsa_cmp_wv2", [256, 64])
    wo_d = din("nsa_w_o", [D, D])
    q2_d = nc.dram_tensor("q2_scratch", [S, 3072], BF16).ap()
    cst = {k: din("c_" + k, v) for k, v in const_shapes(S).items()}
    out_d = nc.dram_tensor("out", [n_seq, S, D], F32, kind="ExternalOutput").ap()

    with contextlib.ExitStack() as es:
        def sb(name, shape, dt):
            return es.enter_context(nc.sbuf_tensor(name, list(shape), dt))

        x_sb = sb("x_sb", [128, NT, D], F32)
        ARENA_E = 52224
        arena = sb("arena", [128, ARENA_E], BF16)
        xnT = sb("xnT", [128, KC, 1024], BF16)
        gbc = sb("gbc", [128, D], F32)
        xs = sb("xs", [128, 2, D], BF16)
        junk = sb("junk", [128, D], BF16)
        sg = sb("sg", [128, 2, 512], F32)
        tmp = sb("tmp", [128, D], F32)
        gcol = sb("gcol", [128, 12, KC], F32)
        ident_f = sb("ident_f", [128, 128], F32)
        ident_b = sb("ident_b", [128, 128], BF16)
        ms = sb("ms", [128, 16], F32)
        std = sb("std", [128, 16], F32)
        rstd = sb("rstd", [128, 16], F32)
        ms2 = sb("ms2", [128, 2], F32)
        std2 = sb("std2", [128, 2], F32)
        rstd2 = sb("rstd2", [128, 2], F32)
        epsb = sb("epsb", [128, 2], F32)
        invcnt = sb("invcnt", [128, 4, 16], F32)
        cs_sb = sb("cs_sb", [128, NT, 16], F32)
        addtbl = sb("addtbl", [128, NT, 32], F32)
        cm3 = sb("cm3", [128, 3, 128], BF16)
        ones_b = sb("ones_b", [128, 128], BF16)
        psum = es.enter_context(nc.psum_tensor("psum", [128, 8, 512], F32))

        p = Prog(nc)
        BK = [Buf("bank%d" % i) for i in range(8)]
        B = {}

        def buf(name):
            if name not in B:
                B[name] = Buf(name)
            return B[name]

        def ps_bf(bank):
            return psum[:, bank, :].bitcast(BF16)

        def aview(off, shape, dt=BF16):
            n = int(np.prod(shape))
            if dt == F32:
                v = arena[:, off:off + 2 * n].bitcast(F32)
                used = 2 * n
            else:
                v = arena[:, off:off + n]
                used = n
            assert off + used <= ARENA_E, (off, used)
            if len(shape) == 2:
                v = v.rearrange("p (a b) -> p a b", a=shape[0])
            elif len(shape) == 3:
                v = v.rearrange("p (a b c) -> p a b c", a=shape[0], b=shape[1])
            return v, off + used

        hT, o1 = aview(0, [FC, 1024])
        wd_sb, o2 = aview(o1, [FC, D])
        NSLOT = 2
        wgu = []
        o3 = o2
        for sl in range(NSLOT):
            a, o3 = aview(o3, [KC, 128])
            b_, o3 = aview(o3, [KC, 128])
            wgu.append((a, b_))

        p.op("pool", lambda e: e.memset(epsb[:, 0:1], EPS), writes=[buf("epsb")])
        p.op("pool", lambda e: e.memset(epsb[:, 1:2], 4 * EPS), writes=[buf("epsb")])
        p.dma("sp", "cst", lambda e: e.dma_start(out=ident_f[:], in_=cst["ident"]), writes=[buf("ident_f")])
        p.dma("sp", "cst", lambda e: e.dma_start(out=invcnt[:], in_=cst["invcnt"].rearrange("p (g t) -> p g t", g=4)),
              writes=[buf("invcnt")])
        p.dma("sp", "cst", lambda e: e.dma_start(out=cs_sb[:], in_=cst["cs"].rearrange("(t p) c -> p t c", p=128)), writes=[buf("cs_sb")])
        p.dma("sp", "cst", lambda e: e.dma_start(out=addtbl[:], in_=cst["addtbl"].rearrange("(t p) c -> p t c", p=128)), writes=[buf("addtbl")])
        p.dma("pool", "cst2", lambda e: e.dma_start(out=cm3[:], in_=cst["cm3"].rearrange("p (a b) -> p a b", a=3)), writes=[buf("cm3")])
        p.op("pool", lambda e: e.memset(ones_b[:], 1.0), writes=[buf("ones_b")])
        p.op("dve", lambda e: e.tensor_copy(out=ident_b[:], in_=ident_f[:]), reads=[buf("ident_f")], writes=[buf("ident_b")])
        for gi in range(12):
            p.dma("sp", "cst", lambda e, gi=gi: e.dma_start(out=gcol[:, gi, :], in_=gains_d[gi].rearrange("(k p) -> p k", p=128),
                                                           allow_slow_non_contiguous=True), writes=[buf("gcol")])

        xs_i = [0]

        def prenorm_tiles(tiles, gi, dst, dst_buf, dst_col0):
            n = len(tiles)
            for i, t in enumerate(tiles):
                p.op("act", lambda e, t=t, i=i: e.activation(out=junk[:], in_=x_sb[:, t, :], func=AF.Square, scale=1.0 / 32,
                                                           accum_out=ms[:, i:i + 1]),
                     reads=[buf("x%d" % t)], writes=[buf("junk"), buf("ms")])
            p.op("act", lambda e: e.activation(out=std[:, 0:n], in_=ms[:, 0:n], func=AF.Sqrt, bias=epsb[:, 0:1], scale=1.0),
                 reads=[buf("ms"), buf("epsb")], writes=[buf("std")])
            p.op("dve", lambda e: e.reciprocal(out=rstd[:, 0:n], in_=std[:, 0:n]), reads=[buf("std")], writes=[buf("rstd")])
            for i, t in enumerate(tiles):
                j = xs_i[0] % 2
                xs_i[0] += 1
                bank = 6 + j
                p.op("act", lambda e, t=t, i=i, j=j: e.activation(out=xs[:, j, :], in_=x_sb[:, t, :], func=AF.Copy, scale=rstd[:, i:i + 1]),
                     reads=[buf("x%d" % t), buf("rstd")], writes=[buf("xs%d" % j)])

                def tr(e, j=j, bank=bank):
                    tpv = ps_bf(bank).rearrange("p (k c) -> p k c", k=KC)
                    for k in range(KC):
                        ins = e.transpose(out=tpv[:, k, :], in_=xs[:, j, k * 128:(k + 1) * 128], identity=ident_b[:])
                    return ins
                p.op("pe", tr, reads=[buf("xs%d" % j), buf("ident_b")], writes=[BK[bank]])
                c0 = dst_col0 + i * 128
                p.op("dve", lambda e, bank=bank, c0=c0: e.tensor_tensor(
                    out=dst[:, :, c0:c0 + 128], in0=ps_bf(bank).rearrange("p (k c) -> p k c", k=KC),
                    in1=gcol[:, gi, :].unsqueeze(2).to_broadcast([128, KC, 128]), op=ALU.mult),
                    reads=[BK[bank], buf("gcol")], writes=[dst_buf])

        def load_gbc(gi):
            p.dma("sp", "gbc", lambda e: e.dma_start(out=gbc[:], in_=gains_d[gi].partition_broadcast(128)), writes=[buf("gbc")])

        def postnorm_add(src_ap, src_bufs, t, half, src_is_psum=True):
            col = 1 if half else 0
            sc = (1.0 / 16) if half else (1.0 / 32)
            p.op("act", lambda e: e.activation(out=junk[:], in_=src_ap, func=AF.Square, scale=sc, accum_out=ms2[:, 0:1]),
                 reads=src_bufs, writes=[buf("junk"), buf("ms2")])
            p.op("act", lambda e: e.activation(out=std2[:, 0:1], in_=ms2[:, 0:1], func=AF.Sqrt, bias=epsb[:, col:col + 1], scale=1.0),
                 reads=[buf("ms2"), buf("epsb")], writes=[buf("std2")])
            p.op("dve", lambda e: e.reciprocal(out=rstd2[:, 0:1], in_=std2[:, 0:1]), reads=[buf("std2")], writes=[buf("rstd2")])
            p.op("act", lambda e: e.activation(out=tmp[:], in_=src_ap, func=AF.Copy, scale=rstd2[:, 0:1]),
                 reads=list(src_bufs) + [buf("rstd2")], writes=[buf("tmp")])
            p.op("dve", lambda e: e.tensor_tensor(out=tmp[:], in0=tmp[:], in1=gbc[:], op=ALU.mult),
                 reads=[buf("tmp"), buf("gbc")], writes=[buf("tmp")])
            p.op("dve", lambda e: e.tensor_tensor(out=x_sb[:, t, :], in0=x_sb[:, t, :], in1=tmp[:], op=ALU.add),
                 reads=[buf("tmp"), buf("x%d" % t)], writes=[buf("x%d" % t)])

        wchunk_ctr = [0]

        def ffn(widx, gi_pre, gi_post):
            load_gbc(gi_post)
            for ps_i in range(2):
                tiles = list(range(ps_i * 8, ps_i * 8 + 8))
                prenorm_tiles(tiles, gi_pre, xnT, buf("xnT"), 0)
                for hh in range(2):
                    p.dma("pool", "wd", lambda e, hh=hh: e.dma_start(
                        out=wd_sb[:, hh * 11:(hh + 1) * 11, :],
                        in_=wd_d[widx, hh * 1408:(hh + 1) * 1408, :].rearrange("(c p) d -> p c d", p=128)),
                        writes=[buf("wd%d" % hh)])
                def load_w(f):
                    sl = wchunk_ctr[0] % NSLOT
                    wchunk_ctr[0] += 1
                    p.dma("pool", "wg%d" % sl, lambda e: e.dma_start(
                        out=wgu[sl][0], in_=wg_d[widx, :, f * 128:(f + 1) * 128].rearrange("(k p) f -> p k f", p=128)),
                        writes=[buf("wg%d" % sl)])
                    p.dma("pool", "wu%d" % sl, lambda e: e.dma_start(
                        out=wgu[sl][1], in_=wu_d[widx, :, f * 128:(f + 1) * 128].rearrange("(k p) f -> p k f", p=128)),
                        writes=[buf("wu%d" % sl)])
                    return sl
                slots = {}
                slots[0] = load_w(0)
                for f in range(FC):
                    if f + 1 < FC:
                        slots[f + 1] = load_w(f + 1)
                    sl = slots[f]
                    for half in range(2):
                        cs_ = slice(half * 512, (half + 1) * 512)

                        def mmg(e, sl=sl, cs_=cs_, half=half):
                            for k in range(KC):
                                ins = e.matmul(psum[:, half, :], lhsT=wgu[sl][0][:, k, :], rhs=xnT[:, k, cs_], start=(k == 0), stop=(k == KC - 1))
                            return ins

                        def mmu(e, sl=sl, cs_=cs_, half=half):
                            for k in range(KC):
                                ins = e.matmul(psum[:, 2 + half, :], lhsT=wgu[sl][1][:, k, :], rhs=xnT[:, k, cs_], start=(k == 0), stop=(k == KC - 1))
                            return ins
                        p.op("pe", mmg, reads=[buf("wg%d" % sl), buf("xnT")], writes=[BK[half]])
                        p.op("pe", mmu, reads=[buf("wu%d" % sl), buf("xnT")], writes=[BK[2 + half]])
                        p.op("act", lambda e, half=half: e.activation(out=sg[:, half, :], in_=psum[:, half, :], func=AF.Silu),
                             reads=[BK[half]], writes=[buf("sg%d" % half)])
                        p.op("dve", lambda e, half=half, f=f, cs_=cs_: e.tensor_tensor(out=hT[:, f, cs_], in0=sg[:, half, :], in1=psum[:, 2 + half, :], op=ALU.mult),
                             reads=[buf("sg%d" % half), BK[2 + half]], writes=[buf("hT%d" % f)])
                hbufs = [buf("hT%d" % f) for f in range(FC)]
                for tt in range(8):
                    t = ps_i * 8 + tt
                    yb = (4, 5) if tt % 2 == 0 else (0, 1)
                    for dh in range(2):
                        def mmy(e, tt=tt, dh=dh, yb=yb):
                            for f in range(FC):
                                ins = e.matmul(psum[:, yb[dh], :], lhsT=hT[:, f, tt * 128:(tt + 1) * 128], rhs=wd_sb[:, f, dh * 512:(dh + 1) * 512],
                                               start=(f == 0), stop=(f == FC - 1))
                            return ins
                        p.op("pe", mmy, reads=hbufs + [buf("wd0"), buf("wd1")], writes=[BK[yb[dh]]])
                    postnorm_add(psum[:, yb[0]:yb[0] + 2, :].rearrange("p a b -> p (a b)"), [BK[yb[0]], BK[yb[1]]], t, half=True)

        def pool_mixer():
            gi_pre, gi_post = 2, 3
            p.barrier()
            hT_seq, o = aview(0, [KC, S])
            sA = []
            for i in range(4):
                v, o = aview(o, [1, S], F32)
                sA.append(v)
            dT, o = aview(o, [KC, S])
            pw, o = aview(o, [4, 2, 256])
            x4 = xnT[:].rearrange("p k c -> p (k c)").bitcast(F32).rearrange("p (a d) -> p a d", a=4)
            bbc, sbc = x4[:, 0, :], x4[:, 1, :]
            load_gbc(gi_post)
            p.dma("sp", "pbc", lambda e: e.dma_start(out=bbc, in_=poolb_d.partition_broadcast(128)), writes=[buf("bbc")])
            p.dma("sp", "pbc", lambda e: e.dma_start(out=sbc, in_=pools_d.partition_broadcast(128)), writes=[buf("sbc")])
            p.dma("pool", "pw", lambda e: e.dma_start(out=pw, in_=poolw_d.rearrange("g (c p) e -> p g c e", p=128)), writes=[buf("pw")])
            for half in range(2):
                prenorm_tiles(list(range(half * 8, half * 8 + 8)), gi_pre, hT_seq, buf("hTs"), half * 1024)
            for c in range(KC):
                g = c // 2
                w = 2 << g
                eng = "dve" if c % 2 == 0 else "pool"
                bufs2 = (sA[0], sA[1]) if eng == "dve" else (sA[2], sA[3])
                nm = ("sA0", "sA1") if eng == "dve" else ("sA2", "sA3")
                a = hT_seq[:, c, :]
                cur, curbuf = a, buf("hTs")
                sh = 1
                lvl = 0
                while sh < w:
                    dst = bufs2[lvl % 2][:, 0, :]
                    dbuf = buf(nm[lvl % 2])
                    p.op(eng, lambda e, dst=dst, cur=cur, sh=sh: e.tensor_tensor(out=dst[:, sh:], in0=cur[:, sh:], in1=cur[:, :S - sh], op=ALU.add),
                         reads=[curbuf], writes=[dbuf])
                    p.op(eng, lambda e, dst=dst, cur=cur, sh=sh: e.tensor_copy(out=dst[:, 0:sh], in_=cur[:, 0:sh]),
                         reads=[curbuf], writes=[dbuf])
                    cur, curbuf = dst, dbuf
                    sh *= 2
                    lvl += 1
                dcb = buf("dT%d" % c)
                p.op("dve", lambda e, cur=cur, a=a, c=c, w=w: e.scalar_tensor_tensor(out=dT[:, c, :], in0=cur, scalar=1.0 / w, in1=a,
                                                                              op0=ALU.mult, op1=ALU.subtract),
                     reads=[curbuf, buf("hTs")], writes=[dcb])
                scr = bufs2[lvl % 2][:, 0, 0:16]
                sbuf_ = buf(nm[lvl % 2])
                p.op(eng, lambda e, cur=cur, g=g, w=w, scr=scr: e.tensor_tensor(out=scr[:, 0:w - 1], in0=cur[:, 0:w - 1], in1=invcnt[:, g, 0:w - 1], op=ALU.mult),
                     reads=[curbuf, buf("invcnt")], writes=[sbuf_])
                p.op(eng, lambda e, a=a, c=c, w=w, scr=scr: e.tensor_tensor(out=dT[:, c, 0:w - 1], in0=scr[:, 0:w - 1], in1=a[:, 0:w - 1], op=ALU.subtract),
                     reads=[sbuf_, buf("hTs")], writes=[dcb])
            dbufs = [buf("dT%d" % c) for c in range(KC)]
            for t in range(NT):
                yb = (4, 5) if t % 2 == 0 else (0, 1)
                for bk in range(2):
                    def mm(e, t=t, bk=bk, yb=yb):
                        for gg in range(2):
                            g = bk * 2 + gg
                            for cc in range(2):
                                ins = e.matmul(psum[:, yb[bk], gg * 256:(gg + 1) * 256], lhsT=dT[:, 2 * g + cc, t * 128:(t + 1) * 128],
                                               rhs=pw[:, g, cc, :], start=(cc == 0), stop=(cc == 1))
                        return ins
                    p.op("pe", mm, reads=dbufs + [buf("pw")], writes=[BK[yb[bk]]])
                m_sb = sA[0][:, 0, 0:1024] if t % 2 == 0 else sA[1][:, 0, 0:1024]
                mb = buf("sA0") if t % 2 == 0 else buf("sA1")
                p.op("dve", lambda e, yb=yb, m_sb=m_sb: e.tensor_tensor(out=m_sb, in0=psum[:, yb[0]:yb[0] + 2, :].rearrange("p a b -> p (a b)"), in1=bbc, op=ALU.add),
                     reads=[BK[yb[0]], BK[yb[1]], buf("bbc")], writes=[mb])
                p.op("pool", lambda e, m_sb=m_sb: e.tensor_tensor(out=m_sb, in0=m_sb, in1=sbc, op=ALU.mult),
                     reads=[mb, buf("sbc")], writes=[mb])
                postnorm_add(m_sb, [mb], t, half=False)
            p.barrier()

        def nsa_mixer():
            gi_pre, gi_post = 8, 9
            p.barrier()
            o = 0
            kswT, o = aview(o, [4, S])
            v1, o = aview(o, [NT, 2, 260])
            gates, o = aview(o, [NT, 48], F32)
            kcmpT2, o = aview(o, [4, 128])
            vaug2, o = aview(o, [4, 98])
            kcmpT, vaug = kcmpT2, vaug2
            o_persist = o
            cT, o = aview(o, [4, S])
            o_n1b = o
            win_sb, o = aview(o, [KC, 2608])
            q2st, o = aview(o, [1, 3072])
            kvst, o = aview(o, [4, 128])
            kswst, o = aview(o, [4, 128])
            load_gbc(gi_post)
            for kk in range(KC):
                p.dma("pool", "win", lambda e, kk=kk: e.dma_start(out=win_sb[:, kk, :], in_=win_d[kk * 128:(kk + 1) * 128, :]), writes=[buf("win")])
            p.op("pool", lambda e: e.memset(v1[:].rearrange("p a b c -> p (a b c)"), 1.0), writes=[buf("v1")])
            rt = tmp[:].rearrange("p (a b) -> p a b", a=4)

            def rope(src, dst, nh, dup):
                cosb = cs_sb[:, T_[0], 0:8].unsqueeze(1).to_broadcast([128, nh, 8])
                sinb = cs_sb[:, T_[0], 8:16].unsqueeze(1).to_broadcast([128, nh, 8])
                t1 = rt[:, 0, 0:nh * 8].rearrange("p (h c) -> p h c", h=nh)
                t2 = rt[:, 1, 0:nh * 8].rearrange("p (h c) -> p h c", h=nh)
                t3 = rt[:, 2, 0:nh * 8].rearrange("p (h c) -> p h c", h=nh)
                t4 = rt[:, 3, 0:nh * 8].rearrange("p (h c) -> p h c", h=nh)
                rd = [BK[b] for b in src[1]] + [buf("cs_sb")]
                sv = src[0]
                bw = [BK[b] for b in src[1]]
                p.op("dve", lambda e: e.tensor_tensor(out=t1, in0=sv[:, :, 0:8], in1=cosb, op=ALU.mult), reads=rd, writes=[buf("tmp")] + bw)
                p.op("dve", lambda e: e.tensor_tensor(out=t2, in0=sv[:, :, 8:16], in1=sinb, op=ALU.mult), reads=rd, writes=[buf("tmp")] + bw)
                p.op("dve", lambda e: e.tensor_tensor(out=t3, in0=sv[:, :, 0:8], in1=sinb, op=ALU.mult), reads=rd, writes=[buf("tmp")] + bw)
                p.op("dve", lambda e: e.tensor_tensor(out=t4, in0=sv[:, :, 8:16], in1=cosb, op=ALU.mult), reads=rd, writes=[buf("tmp")] + bw)
                for dv, db in dst:
                    p.op("dve", lambda e, dv=dv: e.tensor_tensor(out=dv[:, :, 0:8], in0=t1, in1=t2, op=ALU.subtract), reads=[buf("tmp")], writes=[db])
                    p.op("dve", lambda e, dv=dv: e.tensor_tensor(out=dv[:, :, 8:16], in0=t3, in1=t4, op=ALU.add), reads=[buf("tmp")], writes=[db])
                    p.op("act", lambda e, dv=dv: e.activation(out=dv[:, :, 16:64], in_=sv[:, :, 16:64], func=AF.Copy), reads=rd, writes=[db] + bw)

            T_ = [0]
            for t in range(NT):
                T_[0] = t
                prenorm_tiles([t], gi_pre, xnT, buf("xnT"), 0)
                for cb in range(6):
                    wdt = 512 if cb < 5 else 48

                    def mm(e, cb=cb, wdt=wdt):
                        for k in range(KC):
                            ins = e.matmul(psum[:, cb, 0:wdt], lhsT=xnT[:, k, 0:128], rhs=win_sb[:, k, cb * 512:cb * 512 + wdt], start=(k == 0), stop=(k == KC - 1))
                        return ins
                    p.op("pe", mm, reads=[buf("xnT"), buf("win")], writes=[BK[cb]])
                q_ps = psum[:, 0:2, :].rearrange("p a b -> p (a b)")
                q2b = buf("q2st")
                p.op("act", lambda e: e.activation(out=q2st[:, 0, 0:1024], in_=q_ps, func=AF.Copy), reads=[], writes=[q2b, BK[0], BK[1]])
                qd = q2st[:, 0, 1024:3072].rearrange("p (h two c) -> p h two c", h=16, two=2)
                rope((q_ps.rearrange("p (h c) -> p h c", h=16), (0, 1)), [(qd[:, :, 0, :], q2b), (qd[:, :, 1, :], q2b)], 16, True)
                p.dma("sp", "q2w", lambda e, t=t: e.dma_start(out=q2_d[t * 128:(t + 1) * 128, :], in_=q2st[:, 0, :]), reads=[q2b], writes=[buf("q2d%d" % t)])
                p.op("act", lambda e: e.activation(out=kvst[:, :, 0:64], in_=psum[:, 2, 0:256].rearrange("p (g c) -> p g c", g=4), func=AF.Copy), reads=[], writes=[buf("kvst"), BK[2]])
                p.op("act", lambda e: e.activation(out=kvst[:, :, 64:128], in_=psum[:, 2, 256:512].rearrange("p (g c) -> p g c", g=4), func=AF.Copy), reads=[], writes=[buf("kvst"), BK[2]])
                rope((psum[:, 3, 0:256].rearrange("p (h c) -> p h c", h=4), (3,)), [(kswst[:, :, 0:64], buf("kswst"))], 4, False)
                rope((psum[:, 4, 0:256].rearrange("p (h c) -> p h c", h=4), (4,)), [(kswst[:, :, 64:128], buf("kswst"))], 4, False)
                for br, bank in ((0, 3), (1, 4)):
                    p.op("act", lambda e, br=br, bank=bank, t=t: e.activation(
                        out=v1[:, t, br, :].rearrange("p (g c) -> p g c", g=4)[:, :, 0:64],
                        in_=psum[:, bank, 256:512].rearrange("p (g c) -> p g c", g=4), func=AF.Copy), reads=[], writes=[buf("v1"), BK[bank]])
                p.op("act", lambda e, t=t: e.activation(out=gates[:, t, :], in_=psum[:, 5, 0:48], func=AF.Sigmoid), reads=[BK[5]], writes=[buf("gates")])
                def tr(e):
                    tpv = ps_bf(7).rearrange("p (k c) -> p k c", k=8)
                    for g in range(4):
                        e.transpose(out=tpv[:, g, :], in_=kvst[:, g, :], identity=ident_b[:])
                    for g in range(4):
                        ins = e.transpose(out=tpv[:, 4 + g, :], in_=kswst[:, g, :], identity=ident_b[:])
                    return ins
                p.op("pe", tr, reads=[buf("kvst"), buf("kswst"), buf("ident_b")], writes=[BK[7]])
                tpv7 = ps_bf(7).rearrange("p (k c) -> p k c", k=8)
                p.op("dve", lambda e, t=t: e.tensor_copy(out=cT[:, :, t * 128:(t + 1) * 128], in_=tpv7[:, 0:4, :]), reads=[BK[7]], writes=[buf("cT")])
                p.op("dve", lambda e, t=t: e.tensor_copy(out=kswT[:, :, t * 128:(t + 1) * 128], in_=tpv7[:, 4:8, :]), reads=[BK[7]], writes=[buf("kswT")])
            p.barrier()

            o = o_n1b
            wkv1, o = aview(o, [32, 256])
            wkv2, o = aview(o, [2, 2, 64])
            posT, o = aview(o, [1, 32])
            hidT, o = aview(o, [2, 128])
            cbias, o = aview(o, [1, 4], F32)
            p.dma("pool", "w1", lambda e: e.dma_start(out=wkv1[0:64, :, :], in_=wk1_d.rearrange("(t d) h -> d t h", d=64)), writes=[buf("wkv1")])
            p.dma("pool", "w1", lambda e: e.dma_start(out=wkv1[64:128, :, :], in_=wv1_d.rearrange("(t d) h -> d t h", d=64)), writes=[buf("wkv1")])
            p.dma("pool", "w1", lambda e: e.dma_start(out=wkv2[:, 0, :, :], in_=wk2_d.rearrange("(c p) d -> p c d", p=128)), writes=[buf("wkv2")])
            p.dma("pool", "w1", lambda e: e.dma_start(out=wkv2[:, 1, :, :], in_=wv2_d.rearrange("(c p) d -> p c d", p=128)), writes=[buf("wkv2")])
            p.dma("sp", "posf", lambda e: e.dma_start(out=sg[0:64, 0, 0:32], in_=posk_d.rearrange("t d -> d t"), allow_slow_non_contiguous=True), writes=[buf("sg0")])
            p.dma("sp", "posf", lambda e: e.dma_start(out=sg[64:128, 0, 0:32], in_=posv_d.rearrange("t d -> d t"), allow_slow_non_contiguous=True), writes=[buf("sg0")])
            p.op("dve", lambda e: e.tensor_copy(out=posT[:, 0, :], in_=sg[:, 0, 0:32]), reads=[buf("sg0")], writes=[buf("posT")])
            for g in range(4):
                p.dma("pool", "w1", lambda e, g=g: e.dma_start(out=vaug[:, g, 64:97], in_=cst["ovl"]), writes=[buf("vaug")])
            NCMP = (S - 32) // 16 + 1
            for kv in range(2):
                lo, hi = kv * 64, kv * 64 + 64
                for hc in range(2):
                    def mmb(e, lo=lo, hi=hi, hc=hc):
                        for tt in range(32):
                            ins = e.matmul(psum[:, 6, 0:1], lhsT=wkv1[lo:hi, tt, hc * 128:(hc + 1) * 128], rhs=posT[lo:hi, 0, tt:tt + 1], start=(tt == 0), stop=(tt == 31))
                        return ins
                    p.op("pe", mmb, reads=[buf("wkv1"), buf("posT")], writes=[BK[6]])
                    p.op("act", lambda e, kv=kv, hc=hc: e.activation(out=cbias[:, 0, kv * 2 + hc:kv * 2 + hc + 1], in_=psum[:, 6, 0:1], func=AF.Copy), reads=[BK[6]], writes=[buf("cbias")])
            for kv in range(2):
                lo, hi = kv * 64, kv * 64 + 64
                for g in range(4):
                    for hc in range(2):
                        bank = hc

                        def mmc(e, lo=lo, hi=hi, hc=hc, g=g, bank=bank):
                            for tt in range(32):
                                ins = e.matmul(psum[:, bank, 0:NCMP], lhsT=wkv1[lo:hi, tt, hc * 128:(hc + 1) * 128],
                                               rhs=cT[lo:hi, g, tt:tt + 16 * (NCMP - 1) + 1:16], start=(tt == 0), stop=(tt == 31))
                            return ins
                        p.op("pe", mmc, reads=[buf("wkv1"), buf("cT")], writes=[BK[bank]])
                        p.op("act", lambda e, kv=kv, hc=hc, bank=bank: e.activation(out=hidT[:, hc, 0:NCMP], in_=psum[:, bank, 0:NCMP], func=AF.Silu,
                                                                                  bias=cbias[:, 0, kv * 2 + hc:kv * 2 + hc + 1], scale=1.0),
                             reads=[BK[bank], buf("cbias")], writes=[buf("hidT%d" % hc)])
                    if kv == 0:
                        def mmk(e, g=g):
                            for hc in range(2):
                                ins = e.matmul(psum[0:64, 2, 0:NCMP], lhsT=wkv2[:, 0, hc, :], rhs=hidT[:, hc, 0:NCMP], start=(hc == 0), stop=(hc == 1))
                            return ins
                        p.op("pe", mmk, reads=[buf("wkv2"), buf("hidT0"), buf("hidT1")], writes=[BK[2]])
                        p.op("dve", lambda e, g=g: e.tensor_copy(out=kcmpT[0:64, g, 0:NCMP], in_=psum[0:64, 2, 0:NCMP]), reads=[BK[2]], writes=[buf("kcmpT")])
                    else:
                        def mmv(e, g=g):
                            for hc in range(2):
                                ins = e.matmul(psum[0:NCMP, 3, 0:64], lhsT=hidT[:, hc, 0:NCMP], rhs=wkv2[:, 1, hc, :], start=(hc == 0), stop=(hc == 1))
                            return ins
                        p.op("pe", mmv, reads=[buf("wkv2"), buf("hidT0"), buf("hidT1")], writes=[BK[3]])
                        p.op("dve", lambda e, g=g: e.tensor_copy(out=vaug[0:NCMP, g, 0:64], in_=psum[0:NCMP, 3, 0:64]), reads=[BK[3]], writes=[buf("vaug")])
            p.barrier()

            o = o_persist
            wo_sb, o = aview(o, [KC, D])
            q2t, o = aview(o, [1, 3072])
            qT_a, o = aview(o, [8, 128])
            e_s, o = aview(o, [NT, 512])
            e_w, o = aview(o, [3, 512])
            e_c, o = aview(o, [1, 512])
            selx, o = aview(o, [32, 64])
            maskT_a, o = aview(o, [NT, 128])
            o_tile, o = aview(o, [1, D])
            oT_sb, o = aview(o, [KC, 128])
            obr_a, o = aview(o, [3, 256], F32)
            obr_b, o = aview(o, [3, 256], F32)
            imp4, o = aview(o, [4, 32], F32)
            imp, o = aview(o, [1, 32], F32)
            rs4, o = aview(o, [3, 4], F32)
            rsg, o = aview(o, [3, 4], F32)
            mx8, o = aview(o, [1, 8], F32)
            selb, o = aview(o, [1, 32])
            xf = xnT[:].rearrange("p k c -> p (k c)")
            cmpm = xf[:, 0:S].rearrange("p (a b) -> p a b", a=1)
            maskT_b = xf[:, 2048:2048 + NT * 128].rearrange("p (a b) -> p a b", a=NT)
            qT_b = xf[:, 4096:5120].rearrange("p (a b) -> p a b", a=8)
            qT2 = [qT_a, qT_b]
            maskT2 = [maskT_a, maskT_b]
            obr2 = [obr_a, obr_b]
            p.dma("pool", "cmpm", lambda e: e.dma_start(out=cmpm[:, 0, :], in_=cst["cmpmask"]), writes=[buf("cmpm")])
            for kk in range(KC):
                p.dma("pool", "wo", lambda e, kk=kk: e.dma_start(out=wo_sb[:, kk, :], in_=wo_d[kk * 128:(kk + 1) * 128, :]), writes=[buf("wo")])
            sc_i = [0]
            its = [(qi, g) for qi in range(NT) for g in range(4)]

            def pv_norm(bank, width, br, obr, par, qi, g, guard, extra_imp=False):
                ps4 = psum[:, bank, 0:4 * width].rearrange("p (r c) -> p r c", r=4)
                rb = buf("rs4_%d" % br)
                if guard:
                    p.op("dve", lambda e: e.tensor_scalar(out=rs4[:, br, :], in0=ps4[:, :, 64], scalar1=1e-30, scalar2=0.0, op0=ALU.add, op1=ALU.add), reads=[BK[bank]], writes=[rb])
                    p.op("dve", lambda e: e.reciprocal(out=rs4[:, br, :], in_=rs4[:, br, :]), reads=[rb], writes=[rb])
                else:
                    p.op("dve", lambda e: e.reciprocal(out=rs4[:, br, :], in_=ps4[:, :, 64]), reads=[BK[bank]], writes=[rb])
                p.op("dve", lambda e: e.tensor_tensor(out=rsg[:, br, :], in0=rs4[:, br, :], in1=gates[:, qi, br * 16 + 4 * g:br * 16 + 4 * g + 4], op=ALU.mult),
                     reads=[rb, buf("gates")], writes=[buf("rsg_%d" % br)])
                p.op("dve", lambda e: e.tensor_tensor(out=obr[:, br, :].rearrange("p (r c) -> p r c", r=4), in0=ps4[:, :, 0:64],
                                                      in1=rsg[:, br, :].unsqueeze(2).to_broadcast([128, 4, 64]), op=ALU.mult),
                     reads=[BK[bank], buf("rsg_%d" % br)], writes=[buf("obr%d_%d" % (br, par))])
                if extra_imp:
                    p.op("dve", lambda e: e.tensor_tensor(out=imp4[:], in0=ps4[:, :, 65:97], in1=rs4[:, br, :].unsqueeze(2).to_broadcast([128, 4, 32]), op=ALU.mult),
                         reads=[BK[bank], rb], writes=[buf("imp4")])

            def front(it):
                qi, g = its[it]
                par = it % 2
                qT_sb = qT2[par]
                maskT = maskT2[par]
                qTb = buf("qT_%d" % par)
                mTb = buf("maskT_%d" % par)
                if g == 0:
                    p.dma("sp", "q2r", lambda e: e.dma_start(out=q2t[:, 0, :], in_=q2_d[qi * 128:(qi + 1) * 128, :]), reads=[buf("q2d%d" % qi)], writes=[buf("q2t")])

                def trq(e):
                    tpv = ps_bf(7).rearrange("p (k c) -> p k c", k=8)
                    for r in range(4):
                        h = 4 * g + r
                        e.transpose(out=tpv[0:64, r, :], in_=q2t[:, 0, h * 64:(h + 1) * 64], identity=ident_b[:])
                    for r in range(4):
                        h = 4 * g + r
                        ins = e.transpose(out=tpv[:, 4 + r, :], in_=q2t[:, 0, 1024 + h * 128:1024 + (h + 1) * 128], identity=ident_b[:])
                    return ins
                p.op("pe", trq, reads=[buf("q2t"), buf("ident_b")], writes=[BK[7]])
                tpv7 = ps_bf(7).rearrange("p (k c) -> p k c", k=8)
                p.op("act", lambda e: e.activation(out=qT_sb[0:64, 0:4, :], in_=tpv7[0:64, 0:4, :], func=AF.Copy), reads=[BK[7]], writes=[qTb])
                p.op("act", lambda e: e.activation(out=qT_sb[:, 4:8, :], in_=tpv7[:, 4:8, :], func=AF.Copy), reads=[BK[7]], writes=[qTb])
                qn = qT_sb[0:64, 0:4, :].rearrange("p a b -> p (a b)")
                p.op("pe", lambda e: e.matmul(psum[0:NCMP, 6, :], lhsT=kcmpT2[0:64, g, 0:NCMP], rhs=qn, start=True, stop=True),
                     reads=[buf("kcmpT"), qTb], writes=[BK[6]])
                p.op("act", lambda e: e.activation(out=e_c[0:NCMP, 0, :], in_=psum[0:NCMP, 6, :], func=AF.Exp, scale=0.125), reads=[BK[6]], writes=[buf("e_c")])
                p.op("dve", lambda e: e.tensor_tensor(out=e_c[0:NCMP, 0, :].rearrange("p (r c) -> p r c", r=4), in0=e_c[0:NCMP, 0, :].rearrange("p (r c) -> p r c", r=4),
                                                      in1=cmpm[0:NCMP, 0, qi * 128:(qi + 1) * 128].unsqueeze(1).to_broadcast([NCMP, 4, 128]), op=ALU.mult),
                     reads=[buf("e_c"), buf("cmpm")], writes=[buf("e_c")])

                def pvc(e):
                    for r in range(4):
                        ins = e.matmul(psum[:, 2, r * 97:(r + 1) * 97], lhsT=e_c[0:NCMP, 0, r * 128:(r + 1) * 128], rhs=vaug2[0:NCMP, g, 0:97], start=True, stop=True)
                    return ins
                p.op("pe", pvc, reads=[buf("e_c"), buf("vaug")], writes=[BK[2]])
                pv_norm(2, 97, 0, obr2[par], par, qi, g, True, extra_imp=True)
                p.op("dve", lambda e: e.tensor_tensor(out=imp4[:, 0:2, :], in0=imp4[:, 0:2, :], in1=imp4[:, 2:4, :], op=ALU.add), reads=[buf("imp4")], writes=[buf("imp4")])
                p.op("dve", lambda e: e.tensor_tensor(out=imp[:, 0, :], in0=imp4[:, 0, :], in1=imp4[:, 1, :], op=ALU.add), reads=[buf("imp4")], writes=[buf("imp")])
                p.op("dve", lambda e: e.tensor_tensor(out=imp[:, 0, :], in0=imp[:, 0, :], in1=addtbl[:, qi, :], op=ALU.add), reads=[buf("imp"), buf("addtbl")], writes=[buf("imp")])
                p.op("dve", lambda e: e.max(out=mx8[:, 0, :], in_=imp[:, 0, :]), reads=[buf("imp")], writes=[buf("mx8")])
                p.op("dve", lambda e: e.tensor_scalar(out=selb[:, 0, :], in0=imp[:, 0, :], scalar1=mx8[:, 0, 7:8], scalar2=1.0, op0=ALU.is_ge, op1=ALU.mult),
                     reads=[buf("imp"), buf("mx8")], writes=[buf("selb")])
                nb = 2 * (qi + 1)
                p.op("dve", lambda e: e.tensor_copy(out=selx[:, 0:nb, :], in_=selb[:, 0, 0:nb].unsqueeze(2).to_broadcast([128, nb, 64])),
                     reads=[buf("selb")], writes=[buf("selx")])
                p.op("dve", lambda e: e.tensor_tensor(out=selx[:, 2 * qi:2 * qi + 2, :].rearrange("p a b -> p (a b)"),
                                                      in0=selx[:, 2 * qi:2 * qi + 2, :].rearrange("p a b -> p (a b)"), in1=cm3[:, 0, :], op=ALU.mult),
                     reads=[buf("selx"), buf("cm3")], writes=[buf("selx")])

            def front_b(it):
                qi, g = its[it]
                par = it % 2
                maskT = maskT2[par]
                mTb = buf("maskT_%d" % par)
                sx = selx[:].rearrange("p a b -> p (a b)")

                def trm(e):
                    for kt in range(qi + 1):
                        bank = 5 + kt // 8
                        tpv = ps_bf(bank).rearrange("p (k c) -> p k c", k=8)
                        ins = e.transpose(out=tpv[:, kt % 8, :], in_=sx[:, kt * 128:(kt + 1) * 128], identity=ident_b[:])
                    return ins
                p.op("pe", trm, reads=[buf("selx"), buf("ident_b")], writes=[BK[5], BK[6]] if qi >= 8 else [BK[5]])
                n5 = min(qi + 1, 8)
                p.op("act", lambda e: e.activation(out=maskT[:, 0:n5, :], in_=ps_bf(5).rearrange("p (k c) -> p k c", k=8)[:, 0:n5, :], func=AF.Copy),
                     reads=[BK[5]], writes=[mTb])
                if qi >= 8:
                    n6 = qi + 1 - 8
                    p.op("act", lambda e: e.activation(out=maskT[:, 8:8 + n6, :], in_=ps_bf(6).rearrange("p (k c) -> p k c", k=8)[:, 0:n6, :], func=AF.Copy),
                         reads=[BK[6]], writes=[mTb])

            def back(it):
                qi, g = its[it]
                par = it % 2
                qT_sb = qT2[par]
                maskT = maskT2[par]
                obr = obr2[par]
                qTb = buf("qT_%d" % par)
                mTb = buf("maskT_%d" % par)
                qs = qT_sb[0:64, 4:8, :].rearrange("p a b -> p (a b)")
                qw = qT_sb[64:128, 4:8, :].rearrange("p a b -> p (a b)")
                for kt in range(qi + 1):
                    bk = sc_i[0] % 2
                    sc_i[0] += 1
                    p.op("pe", lambda e, bk=bk, kt=kt: e.matmul(psum[:, bk, :], lhsT=kswT[0:64, g, kt * 128:(kt + 1) * 128], rhs=qs, start=True, stop=True),
                         reads=[buf("kswT"), qTb], writes=[BK[bk]])
                    p.op("act", lambda e, bk=bk, kt=kt: e.activation(out=e_s[:, kt, :], in_=psum[:, bk, :], func=AF.Exp, scale=0.125), reads=[BK[bk]], writes=[buf("e_s%d" % kt)])
                    p.op("pool", lambda e, kt=kt: e.tensor_tensor(out=e_s[:, kt, :].rearrange("p (r c) -> p r c", r=4), in0=e_s[:, kt, :].rearrange("p (r c) -> p r c", r=4),
                                                                 in1=maskT[:, kt, :].unsqueeze(1).to_broadcast([128, 4, 128]), op=ALU.mult),
                         reads=[buf("e_s%d" % kt), mTb], writes=[buf("e_s%d" % kt)])

                def pvs(e):
                    for r in range(4):
                        for kt in range(qi + 1):
                            ins = e.matmul(psum[:, 3, r * 65:(r + 1) * 65], lhsT=e_s[:, kt, r * 128:(r + 1) * 128], rhs=v1[:, kt, 0, g * 65:(g + 1) * 65],
                                           start=(kt == 0), stop=(kt == qi))
                    return ins
                p.op("pe", pvs, reads=[buf("e_s%d" % kt) for kt in range(qi + 1)] + [buf("v1")], writes=[BK[3]])
                pv_norm(3, 65, 1, obr, par, qi, g, False)
                kts = [kt for kt in (qi - 2, qi - 1, qi) if kt >= 0]
                for i, kt in enumerate(kts):
                    bk = sc_i[0] % 2
                    sc_i[0] += 1
                    p.op("pe", lambda e, bk=bk, kt=kt: e.matmul(psum[:, bk, :], lhsT=kswT[64:128, g, kt * 128:(kt + 1) * 128], rhs=qw, start=True, stop=True),
                         reads=[buf("kswT"), qTb], writes=[BK[bk]])
                    p.op("act", lambda e, bk=bk, i=i: e.activation(out=e_w[:, i, :], in_=psum[:, bk, :], func=AF.Exp, scale=0.125), reads=[BK[bk]], writes=[buf("e_w%d" % i)])
                    mi = 1 if kt == qi else (2 if kt == qi - 2 else None)
                    if mi is not None:
                        p.op("pool", lambda e, i=i, mi=mi: e.tensor_tensor(out=e_w[:, i, :].rearrange("p (r c) -> p r c", r=4), in0=e_w[:, i, :].rearrange("p (r c) -> p r c", r=4),
                                                                          in1=cm3[:, mi, :].unsqueeze(1).to_broadcast([128, 4, 128]), op=ALU.mult),
                             reads=[buf("e_w%d" % i), buf("cm3")], writes=[buf("e_w%d" % i)])

                def pvw(e):
                    for r in range(4):
                        for i, kt in enumerate(kts):
                            ins = e.matmul(psum[:, 4, r * 65:(r + 1) * 65], lhsT=e_w[:, i, r * 128:(r + 1) * 128], rhs=v1[:, kt, 1, g * 65:(g + 1) * 65],
                                           start=(i == 0), stop=(i == len(kts) - 1))
                    return ins
                p.op("pe", pvw, reads=[buf("e_w%d" % i) for i in range(len(kts))] + [buf("v1")], writes=[BK[4]])
                pv_norm(4, 65, 2, obr, par, qi, g, False)
                p.op("dve", lambda e: e.tensor_tensor(out=obr[:, 0, :], in0=obr[:, 0, :], in1=obr[:, 1, :], op=ALU.add),
                     reads=[buf("obr0_%d" % par), buf("obr1_%d" % par)], writes=[buf("obr0_%d" % par)])
                p.op("dve", lambda e: e.tensor_tensor(out=o_tile[:, 0, g * 256:(g + 1) * 256], in0=obr[:, 0, :], in1=obr[:, 2, :], op=ALU.add),
                     reads=[buf("obr0_%d" % par), buf("obr2_%d" % par)], writes=[buf("o_tile")])

            def finish(qi):
                def tro(e):
                    tpv = ps_bf(7).rearrange("p (k c) -> p k c", k=8)
                    for k in range(KC):
                        ins = e.transpose(out=tpv[:, k, :], in_=o_tile[:, 0, k * 128:(k + 1) * 128], identity=ident_b[:])
                    return ins
                p.op("pe", tro, reads=[buf("o_tile"), buf("ident_b")], writes=[BK[7]])
                p.op("act", lambda e: e.activation(out=oT_sb[:], in_=ps_bf(7).rearrange("p (k c) -> p k c", k=8), func=AF.Copy), reads=[BK[7]], writes=[buf("oT")])
                for dh in range(2):
                    def mmo(e, dh=dh):
                        for k in range(KC):
                            ins = e.matmul(psum[:, dh, :], lhsT=oT_sb[:, k, :], rhs=wo_sb[:, k, dh * 512:(dh + 1) * 512], start=(k == 0), stop=(k == KC - 1))
                        return ins
                    p.op("pe", mmo, reads=[buf("oT"), buf("wo")], writes=[BK[dh]])
                postnorm_add(psum[:, 0:2, :].rearrange("p a b -> p (a b)"), [BK[0], BK[1]], qi, half=False)

            front(0)
            front_b(0)
            for it in range(len(its)):
                if it + 1 < len(its):
                    front(it + 1)
                back(it)
                if it + 1 < len(its):
                    front_b(it + 1)
                if its[it][1] == 3:
                    finish(its[it][0])
            p.barrier()

        xbufs = [buf("x%d" % t) for t in range(NT)]
        for s in range(n_seq):
            p.dma("sp", "xin", lambda e, s=s: e.dma_start(out=x_sb[:], in_=x_d[s].rearrange("(t p) d -> p t d", p=128)), writes=xbufs)
            for st in plan:
                if st.startswith("ffn"):
                    l, h = int(st[3]), int(st[4])
                    ffn(l * 2 + h, l * 6 + (0 if h == 0 else 4), l * 6 + (1 if h == 0 else 5))
                elif st == "pool":
                    pool_mixer()
                elif st == "nsa":
                    nsa_mixer()
            tok = p.dma("sp", "xout", lambda e, s=s: e.dma_start(out=out_d[s].rearrange("(t p) d -> p t d", p=128), in_=x_sb[:]), reads=xbufs)
        p.wait("sp", [tok])
        p.emit()
    return nc


_NC_CACHE = {}


def kernel(x, norm_gains, ffn_w_gate, ffn_w_up, ffn_w_down, pool_w, pool_b, pool_scale,
           nsa_w_in, nsa_cmp_pos_k, nsa_cmp_pos_v, nsa_cmp_wk1, nsa_cmp_wk2,
           nsa_cmp_wv1, nsa_cmp_wv2, nsa_w_o, _plan=None, _n_seq=SEQ_PER_CORE, _cores=N_CORES, _S=S):
    f = lambda a: np.ascontiguousarray(np.asarray(a, dtype=np.float32))
    x = f(x)[:, :_S]
    key = (tuple(_plan) if _plan else None, _n_seq, _S)
    if key not in _NC_CACHE:
        _NC_CACHE[key] = build_nc(_n_seq, _plan, _S, nw=4)
    nc = _NC_CACHE[key]
    shared = {
        "norm_gains": f(norm_gains).reshape(12, D),
        "ffn_w_gate": f(ffn_w_gate).reshape(4, D, DFF),
        "ffn_w_up": f(ffn_w_up).reshape(4, D, DFF),
        "ffn_w_down": f(ffn_w_down).reshape(4, DFF, D),
        "pool_w": f(pool_w).reshape(4, 256, 256),
        "pool_b": f(pool_b).reshape(D),
        "pool_scale": f(pool_scale).reshape(D),
        "nsa_w_in": f(nsa_w_in).reshape(D, 2608),
        "nsa_cmp_pos_k": f(nsa_cmp_pos_k).reshape(32, 64),
        "nsa_cmp_pos_v": f(nsa_cmp_pos_v).reshape(32, 64),
        "nsa_cmp_wk1": f(nsa_cmp_wk1).reshape(2048, 256),
        "nsa_cmp_wk2": f(nsa_cmp_wk2).reshape(256, 64),
        "nsa_cmp_wv1": f(nsa_cmp_wv1).reshape(2048, 256),
        "nsa_cmp_wv2": f(nsa_cmp_wv2).reshape(256, 64),
        "nsa_w_o": f(nsa_w_o).reshape(D, D),
    }
    for k, v in host_consts(_S).items():
        shared["c_" + k] = v
    in_maps = []
    for c in range(_cores):
        m = dict(shared)
        m["x"] = x[c * _n_seq:(c + 1) * _n_seq]
        in_maps.append(m)
    res = run_bass_kernel_spmd(nc, in_maps, core_ids=list(range(_cores)))
    return np.concatenate([r["out"] for r in res.results], axis=0)
```
